# Optimizing a Trainium2 kernel written in Bass

```python
import math
import jax, jax.numpy as jnp
from jax import lax
import numpy as np

D_MODEL = 4096
BATCH = 2
SEQ = 8192
DEPTH = 4

CHUNK = 64
N_MEM = 256

A_HEADS = 8
A_DK = 128
A_DV = 128
A_WIDTH = A_HEADS * A_DV
B_HEADS = 12
B_DK = 128
B_DV = 128
B_WIDTH = B_HEADS * B_DV
B_CONV = 4
B_QKV = 2 * B_HEADS * B_DK + B_WIDTH
C_HEADS = 6
C_DK = 128
C_DV = 256
C_WIDTH = C_HEADS * C_DV
C_RANK = 16
C_TAU = 16.0
MIX_WIDTH = A_WIDTH + B_WIDTH + C_WIDTH

X_HEADS = 4
X_HEAD_DIM = 128
X_WIDTH = X_HEADS * X_HEAD_DIM

N_EXPERTS = 32
TOP_K = 4
EXPERT_FF = 256
SWIGLU_LIMIT = 7.0
SWIGLU_ALPHA = 1.702

DEEPNORM_ALPHA = (2 * DEPTH) ** 0.25
DEEPNORM_BETA = (8 * DEPTH) ** -0.25
LN_EPS = 1e-5
RMS_EPS = 1e-6

IN_SPLITS = (
    A_HEADS * A_DK,
    A_HEADS * A_DK,
    A_WIDTH,
    A_WIDTH,
    B_QKV,
    B_WIDTH,
    B_HEADS,
    B_HEADS,
    C_HEADS * C_DK,
    C_HEADS * C_DK,
    C_WIDTH,
    C_WIDTH,
    C_RANK,
)
IN_COLS = sum(IN_SPLITS)
IN_OFFSETS = tuple(int(v) for v in np.cumsum(IN_SPLITS)[:-1])

kernel_name = "hybrid_hgrn2_gdn_gla_moe_encoder"


def _layer_norm(x, g, b):
    xf = x.astype(jnp.float32)
    mu = jnp.mean(xf, axis=-1, keepdims=True)
    var = jnp.mean(jnp.square(xf - mu), axis=-1, keepdims=True)
    y = (xf - mu) * lax.rsqrt(var + LN_EPS)
    return (y * g + b).astype(x.dtype)


def _rms_norm(x, g):
    xf = x.astype(jnp.float32)
    y = xf * lax.rsqrt(jnp.mean(jnp.square(xf), axis=-1, keepdims=True) + RMS_EPS)
    return (y * g).astype(x.dtype)


def _l2_normalize(x):
    xf = x.astype(jnp.float32)
    return (xf * lax.rsqrt(jnp.sum(jnp.square(xf), axis=-1, keepdims=True) + RMS_EPS)).astype(x.dtype)


def _heads(t, n_heads):
    bsz, seq, _ = t.shape
    return t.reshape(bsz, seq, n_heads, -1).transpose(0, 2, 1, 3)


def _merge_heads(t):
    bsz, n_heads, seq, d = t.shape
    return t.transpose(0, 2, 1, 3).reshape(bsz, seq, n_heads * d)


def _causal_depthwise_conv(x, w):
    k = w.shape[0]
    return lax.conv_general_dilated(
        x, w[:, None, :], window_strides=(1,), padding=[(k - 1, 0)],
        dimension_numbers=("NWC", "WIO", "NWC"), feature_group_count=x.shape[-1])


def _diag_gated_chunked(q, k, v, log_a):
    out_dtype = v.dtype
    bsz, n_heads, seq, dk = q.shape
    dv = v.shape[-1]
    nc = seq // CHUNK

    def chunks(t):
        t = t.astype(jnp.float32)
        return t.reshape(bsz, n_heads, nc, CHUNK, t.shape[-1]).transpose(2, 0, 1, 3, 4)

    qc, kc, vc = chunks(q), chunks(k), chunks(v)
    bc = jnp.cumsum(chunks(log_a), axis=3)
    causal = jnp.tril(jnp.ones((CHUNK, CHUNK), dtype=bool))[:, :, None]

    def step(state, xs):
        qi, ki, vi, bi = xs
        rel = jnp.exp(jnp.where(causal, bi[:, :, :, None, :] - bi[:, :, None, :, :], -jnp.inf))
        scores = jnp.sum(qi[:, :, :, None, :] * ki[:, :, None, :, :] * rel, axis=-1)
        o = scores @ vi + (qi * jnp.exp(bi)) @ state
        last = bi[:, :, -1:, :]
        state = jnp.exp(last).swapaxes(-1, -2) * state + (ki * jnp.exp(last - bi)).swapaxes(-1, -2) @ vi
        return state, o

    s0 = jnp.zeros((bsz, n_heads, dk, dv), jnp.float32)
    _, o = lax.scan(step, s0, (qc, kc, vc, bc))
    return o.transpose(1, 2, 0, 3, 4).reshape(bsz, n_heads, seq, dv).astype(out_dtype)


def _gated_delta_chunked(q, k, v, beta, g):
    out_dtype = v.dtype
    bsz, n_heads, seq, dk = q.shape
    dv = v.shape[-1]
    nc = seq // CHUNK
    qc = q.astype(jnp.float32).reshape(bsz, n_heads, nc, CHUNK, dk)
    kc = k.astype(jnp.float32).reshape(bsz, n_heads, nc, CHUNK, dk)
    vc = v.astype(jnp.float32).reshape(bsz, n_heads, nc, CHUNK, dv)
    bc = beta.astype(jnp.float32).reshape(bsz, n_heads, nc, CHUNK)
    gc = jnp.cumsum(g.astype(jnp.float32).reshape(bsz, n_heads, nc, CHUNK), axis=-1)

    incl = jnp.tril(jnp.ones((CHUNK, CHUNK), dtype=bool))
    strict = jnp.tril(jnp.ones((CHUNK, CHUNK), dtype=bool), -1)
    gamma = jnp.exp(jnp.where(incl, gc[..., :, None] - gc[..., None, :], -jnp.inf))
    kk = jnp.einsum("bhntd,bhnsd->bhnts", kc, kc)
    m = jnp.where(strict, bc[..., :, None] * kk * gamma, 0.0)
    eye = jnp.eye(CHUNK, dtype=jnp.float32)
    rhs = jnp.concatenate([vc * bc[..., None], kc * (bc * jnp.exp(gc))[..., None]], axis=-1)
    sol = lax.linalg.triangular_solve(eye + m, rhs, left_side=True, lower=True, unit_diagonal=True)
    u, w = sol[..., :dv], sol[..., dv:]
    attn = jnp.einsum("bhntd,bhnsd->bhnts", qc, kc) * gamma
    q_dec = qc * jnp.exp(gc)[..., None]
    k_dec = kc * jnp.exp(gc[..., -1:] - gc)[..., None]
    g_last = jnp.exp(gc[..., -1])

    def step(state, xs):
        q_i, k_i, u_i, w_i, a_i, gl = xs
        v_new = u_i - w_i @ state
        o = q_i @ state + a_i @ v_new
        state = gl[..., None, None] * state + k_i.swapaxes(-1, -2) @ v_new
        return state, o

    s0 = jnp.zeros((bsz, n_heads, dk, dv), jnp.float32)
    xs = (jnp.moveaxis(q_dec, 2, 0), jnp.moveaxis(k_dec, 2, 0), jnp.moveaxis(u, 2, 0),
          jnp.moveaxis(w, 2, 0), jnp.moveaxis(attn, 2, 0), jnp.moveaxis(g_last, 2, 0))
    _, o = lax.scan(step, s0, xs)
    return jnp.moveaxis(o, 0, 2).reshape(bsz, n_heads, seq, dv).astype(out_dtype)


def _hybrid_mixer(x, w_in, lb, hgrn_norm_g, conv_w, a_log, dt_bias, gdn_norm_g,
                  gla_w_up, gla_b_up, gla_norm_g, w_out):
    proj = x @ w_in
    (aq, af, ai, ag, bqkv, bz, bb, ba, cq, ck, cv, cr, clr) = jnp.split(proj, IN_OFFSETS, axis=-1)

    f = lb + (1.0 - lb) * jax.nn.sigmoid(af.astype(jnp.float32))
    oa = _diag_gated_chunked(_heads(jax.nn.silu(aq), A_HEADS), _heads(1.0 - f, A_HEADS),
                             _heads(ai, A_HEADS), _heads(jnp.log(f), A_HEADS))
    oa = _rms_norm(oa, hgrn_norm_g) * jax.nn.silu(_heads(ag, A_HEADS))

    qkv = jax.nn.silu(_causal_depthwise_conv(bqkv, conv_w))
    bq, bk, bv = jnp.split(qkv, [B_HEADS * B_DK, 2 * B_HEADS * B_DK], axis=-1)
    qb = _l2_normalize(_heads(bq, B_HEADS)) * (B_DK ** -0.5)
    kb = _l2_normalize(_heads(bk, B_HEADS))
    beta = jax.nn.sigmoid(bb.astype(jnp.float32)).transpose(0, 2, 1)
    gdec = (-jnp.exp(a_log.astype(jnp.float32))
            * jax.nn.softplus(ba.astype(jnp.float32) + dt_bias.astype(jnp.float32))).transpose(0, 2, 1)
    ob = _gated_delta_chunked(qb, kb, _heads(bv, B_HEADS), beta, gdec)
    ob = _rms_norm(ob, gdn_norm_g) * jax.nn.silu(_heads(bz, B_HEADS))

    log_alpha = jax.nn.log_sigmoid((clr @ gla_w_up + gla_b_up).astype(jnp.float32)) / C_TAU
    oc = _diag_gated_chunked(_heads(cq, C_HEADS) * (C_DK ** -0.5), _heads(ck, C_HEADS),
                             _heads(cv, C_HEADS), _heads(log_alpha, C_HEADS))
    oc = _rms_norm(oc, gla_norm_g) * jax.nn.silu(_heads(cr, C_HEADS))

    o = jnp.concatenate([_merge_heads(oa), _merge_heads(ob), _merge_heads(oc)], axis=-1)
    return o @ w_out


def _memory_cross_attention(x, mem_n, wq, wkv, wo):
    bsz, seq, _ = x.shape
    n_mem = mem_n.shape[1]
    q = (x @ wq).reshape(bsz, seq, X_HEADS, X_HEAD_DIM)
    kv = (mem_n @ wkv).reshape(bsz, n_mem, 2, X_HEADS, X_HEAD_DIM)
    k, v = kv[:, :, 0], kv[:, :, 1]
    s = jnp.einsum("bthd,bmhd->bhtm", q, k).astype(jnp.float32) * (X_HEAD_DIM ** -0.5)
    p = jax.nn.softmax(s, axis=-1).astype(x.dtype)
    o = jnp.einsum("bhtm,bmhd->bthd", p, v).reshape(bsz, seq, X_WIDTH)
    return o @ wo


def _moe(x, w_router, b_router, w_gate, b_gate, w_up, b_up, w_down, b_down):
    bsz, seq, d = x.shape
    xt = x.reshape(-1, d)
    logits = (xt @ w_router + b_router).astype(jnp.float32)
    top_logit, top_idx = lax.top_k(logits, TOP_K)
    top_w = jax.nn.softmax(top_logit, axis=-1)
    comb = jnp.einsum("nk,nke->ne", top_w,
                      jax.nn.one_hot(top_idx, N_EXPERTS, dtype=jnp.float32)).astype(x.dtype)
    gate = jnp.minimum(jnp.einsum("nd,edf->nef", xt, w_gate) + b_gate, SWIGLU_LIMIT)
    lin = jnp.clip(jnp.einsum("nd,edf->nef", xt, w_up) + b_up, -SWIGLU_LIMIT, SWIGLU_LIMIT)
    h = (lin + 1.0) * gate * jax.nn.sigmoid(SWIGLU_ALPHA * gate)
    y = jnp.einsum("nef,efd->nd", h * comb[..., None], w_down) + comb @ b_down
    return y.reshape(bsz, seq, d)


def setup_inputs(seed: int = 0) -> dict:
    key = jax.random.key(seed)
    ks = iter(jax.random.split(key, 48))
    L, D, E, F = DEPTH, D_MODEL, N_EXPERTS, EXPERT_FF

    def nrm(shape, scale):
        return jax.random.normal(next(ks), shape, jnp.float32) * scale

    def gain(shape):
        return 1.0 + nrm(shape, 0.02)

    x = nrm((BATCH, SEQ, D), 1.0)
    mem = nrm((BATCH, N_MEM, D), 1.0)
    w_in = nrm((L, D, IN_COLS), D ** -0.5)
    hgrn_lb_raw = nrm((L, A_HEADS * A_DK), 0.5)
    hgrn_norm_g = gain((L, A_DV))
    gdn_conv_w = nrm((L, B_CONV, B_QKV), B_CONV ** -0.5)
    gdn_a_log = jnp.log(jax.random.uniform(next(ks), (L, B_HEADS), jnp.float32, 1.0, 16.0))
    dt = jnp.exp(jax.random.uniform(next(ks), (L, B_HEADS), jnp.float32, math.log(1e-3), math.log(1e-1)))
    gdn_dt_bias = dt + jnp.log(-jnp.expm1(-dt))
    gdn_norm_g = gain((L, B_DV))
    gla_w_up = nrm((L, C_RANK, C_HEADS * C_DK), C_RANK ** -0.5)
    gla_b_up = nrm((L, C_HEADS * C_DK), 0.1)
    gla_norm_g = gain((L, C_DV))
    w_out = nrm((L, MIX_WIDTH, D), MIX_WIDTH ** -0.5 * DEEPNORM_BETA)
    ln_mix_g = gain((L, D))
    ln_mix_b = nrm((L, D), 0.02)
    mem_ln_g = gain((D,))
    mem_ln_b = nrm((D,), 0.02)
    xattn_wq = nrm((L, D, X_WIDTH), D ** -0.5)
    xattn_wkv = nrm((L, D, 2 * X_WIDTH), D ** -0.5)
    xattn_wo = nrm((L, X_WIDTH, D), X_WIDTH ** -0.5 * DEEPNORM_BETA)
    ln_xattn_g = gain((L, D))
    ln_xattn_b = nrm((L, D), 0.02)
    w_router = nrm((L, D, E), D ** -0.5)
    b_router = nrm((L, E), 0.01)
    w_gate = nrm((L, E, D, F), D ** -0.5)
    b_gate = nrm((L, E, F), 0.02)
    w_up = nrm((L, E, D, F), D ** -0.5)
    b_up = nrm((L, E, F), 0.02)
    w_down = nrm((L, E, F, D), F ** -0.5 * DEEPNORM_BETA)
    b_down = nrm((L, E, D), 0.02)
    ln_ffn_g = gain((L, D))
    ln_ffn_b = nrm((L, D), 0.02)
    return {
        "x": x, "mem": mem, "w_in": w_in, "hgrn_lb_raw": hgrn_lb_raw, "hgrn_norm_g": hgrn_norm_g,
        "gdn_conv_w": gdn_conv_w, "gdn_a_log": gdn_a_log, "gdn_dt_bias": gdn_dt_bias,
        "gdn_norm_g": gdn_norm_g, "gla_w_up": gla_w_up, "gla_b_up": gla_b_up, "gla_norm_g": gla_norm_g,
        "w_out": w_out, "ln_mix_g": ln_mix_g, "ln_mix_b": ln_mix_b, "mem_ln_g": mem_ln_g,
        "mem_ln_b": mem_ln_b, "xattn_wq": xattn_wq, "xattn_wkv": xattn_wkv, "xattn_wo": xattn_wo,
        "ln_xattn_g": ln_xattn_g, "ln_xattn_b": ln_xattn_b, "w_router": w_router, "b_router": b_router,
        "w_gate": w_gate, "b_gate": b_gate, "w_up": w_up, "b_up": b_up, "w_down": w_down,
        "b_down": b_down, "ln_ffn_g": ln_ffn_g, "ln_ffn_b": ln_ffn_b,
    }


def reference(x, mem, w_in, hgrn_lb_raw, hgrn_norm_g, gdn_conv_w, gdn_a_log, gdn_dt_bias,
              gdn_norm_g, gla_w_up, gla_b_up, gla_norm_g, w_out, ln_mix_g, ln_mix_b, mem_ln_g,
              mem_ln_b, xattn_wq, xattn_wkv, xattn_wo, ln_xattn_g, ln_xattn_b, w_router, b_router,
              w_gate, b_gate, w_up, b_up, w_down, b_down, ln_ffn_g, ln_ffn_b):
    lb_all = jnp.cumsum(jax.nn.softmax(hgrn_lb_raw.astype(jnp.float32), axis=0), axis=0)
    mem_n = _layer_norm(mem, mem_ln_g, mem_ln_b)
    for l in range(DEPTH):
        h = _hybrid_mixer(x, w_in[l], lb_all[l] - lb_all[0], hgrn_norm_g[l], gdn_conv_w[l],
                          gdn_a_log[l], gdn_dt_bias[l], gdn_norm_g[l], gla_w_up[l], gla_b_up[l],
                          gla_norm_g[l], w_out[l])
        x = _layer_norm(DEEPNORM_ALPHA * x + h, ln_mix_g[l], ln_mix_b[l])
        h = _memory_cross_attention(x, mem_n, xattn_wq[l], xattn_wkv[l], xattn_wo[l])
        x = _layer_norm(DEEPNORM_ALPHA * x + h, ln_xattn_g[l], ln_xattn_b[l])
        h = _moe(x, w_router[l], b_router[l], w_gate[l], b_gate[l], w_up[l], b_up[l],
                 w_down[l], b_down[l])
        x = _layer_norm(DEEPNORM_ALPHA * x + h, ln_ffn_g[l], ln_ffn_b[l])
    return x
```

```python
import numpy as np
from contextlib import ExitStack
import concourse.bass as bass
import concourse.mybir as mybir
from concourse.bass_utils import run_bass_kernel_spmd

F32 = mybir.dt.float32
BF16 = mybir.dt.bfloat16
AF = mybir.ActivationFunctionType
ALU = mybir.AluOpType
AX = mybir.AxisListType

D = 4096
KC = 32
NCORE = 8
BLK = 256
NTT = BLK // 128
ALPHA = 8.0 ** 0.25
LN_EPS = 1e-5
RMS_EPS = 1e-6
MB = 256
WMIX_GROUPS = [(0, 512), (16384, 576), (34816, 576), (53248, 784)]
WMIX_TOT = 78336
OROWS = 640
SLOT = 4096
NSLOT = 7

DPW = [
    ("wout", 4096, 4096), ("wq", 128, 16384), ("wkv", 128, 32768), ("wo", 128, 16384),
    ("wg", 4096, 8192), ("wu", 4096, 8192), ("wd", 4096, 8192),
]


class Sch:
    def __init__(self, nc, es):
        self.nc = nc
        self.eng = {"pe": nc.tensor, "act": nc.scalar, "dve": nc.vector, "pool": nc.gpsimd, "sp": nc.sync}
        self.stream = {k: [] for k in self.eng}
        self.sems = {}
        self.cnt = {}
        for k in ("pe", "act", "dve", "pool"):
            self.sems[k] = es.enter_context(nc.semaphore("c_" + k))
            self.cnt[k] = 0
        self.dq = {}
        for q, n in (("sp", 12), ("pool", 6), ("act", 2)):
            names = []
            for i in range(n):
                nm = "d_%s%d" % (q, i)
                self.sems[nm] = es.enter_context(nc.semaphore(nm))
                self.cnt[nm] = 0
                names.append(nm)
            self.dq[q] = [names, 0]
        self.sems["cc"] = es.enter_context(nc.semaphore("ccsem"))
        self.cnt["cc"] = 0
        self.seen = {k: {} for k in self.eng}
        self.lastw = {}
        self.rd = {}
        self.nins = 0

    def _deps(self, reads, writes):
        d = {}
        for b in reads:
            w = self.lastw.get(b)
            if w and d.get(w[0], 0) < w[1]:
                d[w[0]] = w[1]
        for b in writes:
            w = self.lastw.get(b)
            if w and d.get(w[0], 0) < w[1]:
                d[w[0]] = w[1]
            for s, v in self.rd.get(b, {}).items():
                if d.get(s, 0) < v:
                    d[s] = v
        return d

    def _waits(self, e, d):
        for s, v in d.items():
            if self.seen[e].get(s, 0) < v:
                self.seen[e][s] = v
                sem = self.sems[s]
                self.stream[e].append(lambda E, sem=sem, v=v: E.wait_ge(sem, v))
                self.nins += 1

    def _mark(self, s, v, reads, writes):
        for b in writes:
            self.lastw[b] = (s, v)
            self.rd[b] = {}
        for b in reads:
            r = self.rd.setdefault(b, {})
            if r.get(s, 0) < v:
                r[s] = v

    def op(self, e, fns, reads=(), writes=()):
        if not isinstance(fns, (list, tuple)):
            fns = [fns]
        self._waits(e, self._deps(reads, writes))
        self.cnt[e] += 1
        v = self.cnt[e]
        sem = self.sems[e]
        st = self.stream[e]
        for f in fns[:-1]:
            st.append(f)
        last = fns[-1]
        st.append(lambda E, last=last, sem=sem: last(E).then_inc(sem, 1))
        self.nins += len(fns)
        self._mark(e, v, reads, writes)

    def dma(self, q, out, in_, reads=(), writes=(), **kw):
        names, idx = self.dq[q]
        s = names[idx % len(names)]
        self.dq[q][1] += 1
        d = self._deps(reads, writes)
        if self.cnt[s] > 0:
            d[s] = max(d.get(s, 0), self.cnt[s])
        self._waits(q, d)
        self.cnt[s] += 16
        v = self.cnt[s]
        sem = self.sems[s]
        self.stream[q].append(lambda E, out=out, in_=in_, sem=sem, kw=kw: E.dma_start(out=out, in_=in_, **kw).then_inc(sem, 16))
        self.nins += 1
        self._mark(s, v, reads, writes)

    def dma_fn(self, q, fn, reads=(), writes=()):
        names, idx = self.dq[q]
        s = names[idx % len(names)]
        self.dq[q][1] += 1
        d = self._deps(reads, writes)
        if self.cnt[s] > 0:
            d[s] = max(d.get(s, 0), self.cnt[s])
        self._waits(q, d)
        self.cnt[s] += 16
        v = self.cnt[s]
        sem = self.sems[s]
        self.stream[q].append(lambda E, fn=fn, sem=sem: fn(E).then_inc(sem, 16))
        self.nins += 1
        self._mark(s, v, reads, writes)

    def allgather(self, in_t, out_t, reads, writes):
        d = self._deps(reads, writes)
        if self.cnt["cc"] > 0:
            d["cc"] = max(d.get("cc", 0), self.cnt["cc"])
        self._waits("pool", d)
        self.cnt["cc"] += 1
        v = self.cnt["cc"]
        sem = self.sems["cc"]
        self.stream["pool"].append(
            lambda E, in_t=in_t, out_t=out_t, sem=sem: E.collective_compute(
                "AllGather", ALU.bypass, replica_groups=[list(range(NCORE))],
                ins=[in_t.ap().opt()], outs=[out_t.ap().opt()]).then_inc(sem))
        self.nins += 1
        self._mark("cc", v, reads, writes)

    def finish(self, final_keys):
        d = self._deps(final_keys, ())
        self._waits("sp", d)
        alld = {s: c for s, c in self.cnt.items() if c > 0}
        self._waits("sp", alld)
        with self.nc.Block() as block:
            @block.tensor
            def _(E):
                for f in self.stream["pe"]:
                    f(E)

            @block.scalar
            def _(E):
                for f in self.stream["act"]:
                    f(E)

            @block.vector
            def _(E):
                for f in self.stream["dve"]:
                    f(E)

            @block.gpsimd
            def _(E):
                for f in self.stream["pool"]:
                    f(E)

            @block.sync
            def _(E):
                for f in self.stream["sp"]:
                    f(E)


def build(TC, NL, dbg=None):
    dbg = dbg or {}
    NB = TC // BLK
    nc = bass.Bass("TRN2", target_bir_lowering=False)
    es = ExitStack()

    def din(name, shape, dt=F32):
        return nc.dram_tensor(name, list(shape), dt, kind="ExternalInput")

    x_in = din("x", [TC, D])
    mem_in = din("mem", [256, D])
    wsh = {n: din("sh_" + n, [NL, r // NCORE, c]) for n, r, c in DPW}
    wr_in = din("wr", [NL, 128, KC * 32])
    br_in = din("br", [NL, 32, 1])
    bg_in = din("bg", [NL, 128, 64])
    bu_in = din("bu", [NL, 128, 64])
    bd_in = din("bd", [NL, 32, D])
    lng_in = din("lng", [NL, 3, 128, KC])
    lnb_in = din("lnb", [NL, 3, 128, KC])
    mlg_in = din("mlg", [128, KC])
    mlb_in = din("mlb", [128, KC])
    ident_in = din("ident", [128, 128])
    sel_in = din("sel", [32, 32 * 128])
    out_d = nc.dram_tensor("out", [TC, D], F32, kind="ExternalOutput")
    NT = NCORE * TC
    wmix_in = din("wmix", [NL, 128, WMIX_TOT])
    convw_in = din("convw", [NL, 2, 128, 12])
    bsc_in = din("bsc", [NL, 2, 64, 2])
    lbT_in = din("lbT", [128, NL])
    ngA_in = din("ngA", [NL, 128, 1])
    ngB_in = din("ngB", [NL, 128, 1])
    ngC_in = din("ngC", [NL, 128, 2])
    glaw_in = din("glaw", [NL, 16, 128])
    glab_in = din("glab", [NL, 128, 1])
    cmask_in = din("cmask", [128, MB])
    maskT_in = din("maskT", [64, 64])
    maskN_in = din("maskN", [64, 64])
    sel2_in = din("sel2", [64, 2])
    wmixb = [nc.dram_tensor("wmixb%d" % l, [128, WMIX_TOT], BF16) for l in range(NL)]
    oT_part = nc.dram_tensor("oT_part", [OROWS, NT], BF16)
    oT_all = nc.dram_tensor("oT_all", [NCORE * OROWS, NT], BF16)
    if "dump_oT" in dbg:
        oT_out = nc.dram_tensor("oT_out", [NCORE * OROWS, TC], BF16, kind="ExternalOutput")
    if "oT" in dbg:
        oT_dbg = din("oT_dbg", [NL, NCORE * OROWS, TC])

    wcast = {(n, l): nc.dram_tensor("c_%s%d" % (n, l), [r // NCORE, c], BF16) for n, r, c in DPW for l in range(NL)}
    wfull = {(n, l): nc.dram_tensor("g_%s%d" % (n, l), [r, c], BF16) for n, r, c in DPW for l in range(NL)}
    xresT = nc.dram_tensor("xresT", [D, TC], F32)
    xT_loc = nc.dram_tensor("xT_loc", [D, TC], BF16)
    xT_all = nc.dram_tensor("xT_all", [NCORE * D, TC], BF16)
    oT_mine = nc.dram_tensor("oT_mine", [NCORE * OROWS, TC], BF16)
    memn_d = nc.dram_tensor("memn_d", [128, KC * 256], BF16)

    def sb(name, shape, dt):
        return es.enter_context(nc.sbuf_tensor(name, list(shape), dt))

    RING = sb("ring", [128, NSLOT * SLOT], BF16)
    G = sb("garena", [128, 32768], BF16)
    XBF = sb("xbf", [128, KC * BLK], BF16)
    KT = sb("kt", [128, 4 * 256], BF16)
    VV = sb("vv", [128, 2 * 512], BF16)
    QT = sb("qt", [128, 4 * BLK], BF16)
    OX = sb("ox", [128, 4 * BLK], BF16)
    SQ = sb("sq", [128, 2 * BLK], F32)
    ST = sb("st", [128, 4 * BLK], F32)
    PSB = sb("psb", [128, 256], F32)
    PN = sb("pn", [128, 256], BF16)
    PT = sb("pt", [128, 2 * 128], BF16)
    SM = sb("sm", [128, 16], F32)
    LG = sb("lg", [128, 4 * 32], F32)
    LGT = sb("lgt", [32, BLK], F32)
    CT = sb("ct", [32, BLK], F32)
    CTB = sb("ctb", [32, BLK], BF16)
    WR = sb("wrt", [128, KC * 32], F32)
    BR = sb("brt", [32, 1], F32)
    BGt = sb("bgt", [128, 64], F32)
    BUt = sb("but", [128, 64], F32)
    BDb = sb("bdb", [32, D], BF16)
    LNG = sb("lngt", [128, 3 * KC], F32)
    LNB = sb("lnbt", [128, 3 * KC], F32)
    MLG = sb("mlgt", [128, KC], F32)
    MLB = sb("mlbt", [128, KC], F32)
    IDF = sb("idf", [128, 128], F32)
    IDB = sb("idb", [128, 128], BF16)
    ONF = sb("onf", [128, 128], F32)
    SELB = sb("selb", [32, 32 * 128], BF16)
    FT = [sb("ft%d" % i, [128, MB + 4], F32) for i in range(10)]
    HB = [sb("hb%d" % i, [128, MB], BF16) for i in range(9)]
    SS = sb("sstate", [128, 256], F32)
    SSB = sb("sstateb", [128, 256], BF16)
    CK = [sb("ck%d" % i, [64, 384], BF16) for i in range(6)]
    CF = [sb("cf%d" % i, [64, 64], F32) for i in range(12)]
    COL = sb("col", [128, 16], F32)
    OTS = sb("ots", [128, 2 * MB], BF16)
    LBT = sb("lbtt", [128, 4 * NL], F32)
    CONVW = sb("convwt", [128, 12], F32)
    BSC = sb("bsct", [64, 4], F32)
    NGT = sb("ngt", [128, 4], F32)
    GLAW = sb("glawt", [16, 128], F32)
    GLAWB = sb("glawb", [16, 128], BF16)
    GLAB = sb("glabt", [128, 2], F32)
    CMASK = sb("cmaskt", [128, MB], F32)
    MASKT = sb("maskt", [64, 64], F32)
    MASKN = sb("maskn", [64, 64], F32)
    SEL2 = sb("sel2t", [64, 2], F32)
    T1, T2, T3 = FT[0][:, 0:BLK], FT[1][:, 0:BLK], FT[2][:, 0:BLK]
    PS = [es.enter_context(nc.psum_tensor("ps%d" % i, [128, 512], F32)) for i in range(8)]

    S = Sch(nc, es)
    XA = G[:, 0:16384].bitcast(F32)
    XA3 = XA.rearrange("p (k t) -> p k t", k=KC)
    G1 = G[:, 16384:32768]
    HT3 = G1.rearrange("p (i t) -> p i t", t=BLK)
    TM = G1.bitcast(F32)
    XBF3 = XBF[:, :].rearrange("p (k t) -> p k t", k=KC)
    MEMN3 = XBF3
    kXA, kG1 = ("G", 0), ("G", 1)
    ring_i = [0]

    def slot():
        i = ring_i[0] % NSLOT
        ring_i[0] += 1
        return RING[:, i * SLOT:(i + 1) * SLOT], ("ring", i)

    psr = [0]

    def psum_main():
        i = psr[0] % 4
        psr[0] += 1
        return PS[i], ("ps", i)

    S.dma("sp", IDF[:, :], ident_in[:, :], writes=["idf"])
    S.dma("pool", SELB[:, :].rearrange("p (a b) -> p a b", b=2048), sel_in[:, :].rearrange("p (a b) -> p a b", b=2048), writes=["selb"])
    S.op("act", lambda E: E.activation(IDB[:, :], IDF[:, :], AF.Copy), reads=["idf"], writes=["idb"])
    S.op("pool", lambda E: E.memset(ONF[:, :], 1.0), writes=["onf"])
    S.dma("sp", MLG[:, :], mlg_in[:, :], writes=["mlg"])
    S.dma("sp", CMASK[:, :], cmask_in[:, :], writes=["cmask"])
    S.dma("sp", MASKT[:, :], maskT_in[:, :], writes=["maskt"])
    S.dma("sp", MASKN[:, :], maskN_in[:, :], writes=["maskn"])
    S.dma("sp", SEL2[:, :], sel2_in[:, :], writes=["sel2"])
    S.dma("sp", LBT[:, 0:NL], lbT_in[:, :], writes=["lbt"])
    S.op("act", lambda E: E.activation(LBT[:, NL:2 * NL], LBT[:, 0:NL], AF.Exp, accum_out=COL[:, 15:16]), reads=["lbt"], writes=["lbt", "col15"])
    S.op("dve", lambda E: E.reciprocal(COL[:, 14:15], COL[:, 15:16]), reads=["col15"], writes=["col14"])
    S.op("dve", lambda E: E.tensor_scalar(LBT[:, NL:2 * NL], LBT[:, NL:2 * NL], COL[:, 14:15], None, ALU.mult), reads=["lbt", "col14"], writes=["lbt"])
    S.op("pool", lambda E: E.memset(LBT[:, 2 * NL:2 * NL + 1], 0.0), reads=["lbt"], writes=["lbt"])
    for l_ in range(1, NL):
        S.op("dve", lambda E, l_=l_: E.tensor_tensor(LBT[:, 2 * NL + l_:2 * NL + l_ + 1], LBT[:, 2 * NL + l_ - 1:2 * NL + l_], LBT[:, NL + l_:NL + l_ + 1], ALU.add),
             reads=["lbt"], writes=["lbt"])
    S.op("dve", lambda E: E.tensor_scalar(LBT[:, 3 * NL:4 * NL], LBT[:, 2 * NL:3 * NL], -1.0, 1.0, ALU.mult, ALU.add), reads=["lbt"], writes=["lbt"])
    for l_ in range(NL):
        for (off, ncols_) in WMIX_GROUPS:
            n_el = 32 * ncols_
            bw = 2048 if n_el % 2048 == 0 else 512
            S.dma("pool", wmixb[l_][:, off:off + n_el].rearrange("p (a b) -> p a b", b=bw),
                  wmix_in[l_][:, off:off + n_el].rearrange("p (a b) -> p a b", b=bw), writes=[("wmixb", l_, off)])
    S.dma("sp", MLB[:, :], mlb_in[:, :], writes=["mlb"])

    for l in range(NL):
        for n, r, c in DPW:
            rows = r // NCORE
            src = wsh[n][l]
            dst = wcast[(n, l)]
            per = max(1, (1 << 21) // c)
            for r0 in range(0, rows, per):
                r1 = min(rows, r0 + per)
                S.dma("pool", dst[r0:r1, :].rearrange("r (a b) -> (r a) b", b=2048),
                      src[r0:r1, :].rearrange("r (a b) -> (r a) b", b=2048),
                      writes=[("wc", n, l, r0)])
            S.allgather(dst, wfull[(n, l)], reads=[("wc", n, l, r0) for r0 in range(0, rows, per)],
                        writes=[("wf", n, l)])

    def ln_fm(g_ap, b_ap, gkeys, out_bf3, out_bf_key, ncols=BLK):
        X3 = XA3[:, :, 0:ncols]
        S.op("pe", [lambda E, k=k: E.matmul(PS[4][:, 0:ncols], ONF[:, :], XA3[:, k, 0:ncols], start=(k == 0), stop=(k == KC - 1))
                    for k in range(KC)], reads=[kXA, "onf"], writes=[("ps", 4)])
        for g2 in range(KC // 2):
            S.op("act", lambda E, g2=g2: E.activation(SQ[:, 0:2 * ncols].rearrange("p (a t) -> p a t", a=2),
                                                      XA3[:, g2 * 2:(g2 + 1) * 2, 0:ncols], AF.Square),
                 reads=[kXA], writes=["sq"])
            S.op("pe", [lambda E, g2=g2, a=a: E.matmul(PS[5][:, 0:ncols], ONF[:, :], SQ[:, a * ncols:(a + 1) * ncols],
                                                      start=(g2 == 0 and a == 0), stop=(g2 == KC // 2 - 1 and a == 1))
                        for a in range(2)], reads=["sq", "onf"], writes=[("ps", 5)])
        mean, var, rstd, nmr = (ST[:, i * BLK:i * BLK + ncols] for i in range(4))
        S.op("dve", lambda E: E.tensor_scalar(mean, PS[4][:, 0:ncols], 1.0 / D, None, ALU.mult), reads=[("ps", 4)], writes=["st0"])
        S.op("dve", lambda E: E.tensor_scalar(var, PS[5][:, 0:ncols], 1.0 / D, None, ALU.mult), reads=[("ps", 5)], writes=["st1"])
        S.op("dve", lambda E: E.tensor_tensor(nmr, mean, mean, ALU.mult), reads=["st0"], writes=["st3"])
        S.op("dve", lambda E: E.tensor_tensor(var, var, nmr, ALU.subtract), reads=["st1", "st3"], writes=["st1"])
        S.op("act", lambda E: E.activation(var, var, AF.Sqrt, bias=LN_EPS), reads=["st1"], writes=["st1"])
        S.op("dve", lambda E: E.reciprocal(rstd, var), reads=["st1"], writes=["st2"])
        S.op("dve", lambda E: E.scalar_tensor_tensor(nmr, mean, -1.0, rstd, ALU.mult, ALU.mult), reads=["st0", "st2"], writes=["st3"])
        for q in range(4):
            ks = slice(q * 8, (q + 1) * 8)
            Xq = XA3[:, ks, 0:ncols]
            eng = "dve" if q % 2 == 0 else "pool"
            S.op(eng, lambda E, Xq=Xq: E.tensor_tensor(Xq, Xq, rstd.unsqueeze(1).broadcast_to([128, 8, ncols]), ALU.mult),
                 reads=[kXA, "st2"], writes=[kXA])
            S.op(eng, lambda E, Xq=Xq: E.tensor_tensor(Xq, Xq, nmr.unsqueeze(1).broadcast_to([128, 8, ncols]), ALU.add),
                 reads=[kXA, "st3"], writes=[kXA])
            S.op(eng, lambda E, Xq=Xq, ks=ks: E.tensor_tensor(Xq, Xq, g_ap[:, ks].unsqueeze(2).broadcast_to([128, 8, ncols]), ALU.mult),
                 reads=[kXA] + gkeys, writes=[kXA])
            S.op(eng, lambda E, Xq=Xq, ks=ks: E.tensor_tensor(Xq, Xq, b_ap[:, ks].unsqueeze(2).broadcast_to([128, 8, ncols]), ALU.add),
                 reads=[kXA] + gkeys, writes=[kXA])
        for q in range(4):
            ks = slice(q * 8, (q + 1) * 8)
            S.op("act", lambda E, ks=ks: E.activation(out_bf3[:, ks, 0:ncols], XA3[:, ks, 0:ncols], AF.Copy),
                 reads=[kXA], writes=[out_bf_key])

    def load_tokmajor_to_fm(src_rows_ap):
        for tt in range(NTT):
            S.dma("sp", TM[:, tt * D:(tt + 1) * D], src_rows_ap[tt * 128:(tt + 1) * 128, :], writes=[kG1])
        for tt in range(NTT):
            for k4 in range(KC // 4):
                ps, pk = psum_main()
                S.op("pe", [lambda E, ps=ps, a=a, k4=k4, tt=tt: E.transpose(ps[:, a * 128:(a + 1) * 128],
                                                                         TM[:, tt * D + (k4 * 4 + a) * 128: tt * D + (k4 * 4 + a + 1) * 128], IDF[:, :])
                            for a in range(4)], reads=[kG1, "idf"], writes=[pk])
                S.op("dve" if k4 % 2 == 0 else "act",
                     (lambda E, ps=ps, k4=k4, tt=tt: E.tensor_copy(XA3[:, k4 * 4:(k4 + 1) * 4, tt * 128:(tt + 1) * 128],
                                                                  ps[:, :].rearrange("p (a t) -> p a t", a=4)))
                     if k4 % 2 == 0 else
                     (lambda E, ps=ps, k4=k4, tt=tt: E.activation(XA3[:, k4 * 4:(k4 + 1) * 4, tt * 128:(tt + 1) * 128],
                                                                  ps[:, :].rearrange("p (a t) -> p a t", a=4), AF.Copy)),
                     reads=[pk], writes=[kXA])


    def store_final(b):
        for tt in range(NTT):
            for k4 in range(KC // 4):
                ps, pk = psum_main()
                S.op("pe", [lambda E, ps=ps, a=a, k4=k4, tt=tt: E.transpose(ps[:, a * 128:(a + 1) * 128], XA3[:, k4 * 4 + a, tt * 128:(tt + 1) * 128], IDF[:, :])
                            for a in range(4)], reads=[kXA, "idf"], writes=[pk])
                S.op("dve" if k4 % 2 == 0 else "act",
                     (lambda E, ps=ps, k4=k4, tt=tt: E.tensor_copy(TM[:, tt * D + k4 * 512: tt * D + (k4 + 1) * 512], ps[:, :]))
                     if k4 % 2 == 0 else
                     (lambda E, ps=ps, k4=k4, tt=tt: E.activation(TM[:, tt * D + k4 * 512: tt * D + (k4 + 1) * 512], ps[:, :], AF.Copy)),
                     reads=[pk], writes=[kG1])
            S.dma("sp", out_d[b * BLK + tt * 128: b * BLK + (tt + 1) * 128, :], TM[:, tt * D:(tt + 1) * D], reads=[kG1], writes=[("out", b, tt)])

    load_tokmajor_to_fm(mem_in)
    ln_fm(MLG, MLB, ["mlg", "mlb"], XBF3, "xbf", ncols=256)
    S.dma("sp", memn_d[:, :], XBF[:, :], reads=["xbf"], writes=["memn_d"])

    for b in range(NB):
        load_tokmajor_to_fm(x_in[b * BLK:(b + 1) * BLK, :])
        S.dma("sp", xresT[:, b * BLK:(b + 1) * BLK].rearrange("(k p) t -> p k t", p=128), XA3, reads=[kXA], writes=[("xres", b)])
        for q in range(4):
            ks = slice(q * 8, (q + 1) * 8)
            S.op("act", lambda E, ks=ks: E.activation(XBF3[:, ks, :], XA3[:, ks, :], AF.Copy), reads=[kXA], writes=["xbf"])
        S.dma("sp", xT_loc[:, b * BLK:(b + 1) * BLK].rearrange("(k p) t -> p k t", p=128), XBF3, reads=["xbf"], writes=[("xtl", b)])

    def PS4b():
        return PS[4][:, :].bitcast(BF16)

    def rms_gate_out(o_ps, okey, dv, gts, ngcols, c):
        cs = slice(c * 64, (c + 1) * 64)
        S.op("act", lambda E: E.activation(FT[8][0:64, 0:dv], o_ps, AF.Square, accum_out=COL[0:64, 0:1]), reads=[okey], writes=["ft8", "col0"])
        S.op("act", lambda E: E.activation(COL[0:64, 1:2], COL[0:64, 0:1], AF.Sqrt, bias=RMS_EPS, scale=1.0 / dv), reads=["col0"], writes=["col1"])
        S.op("dve", lambda E: E.reciprocal(COL[0:64, 2:3], COL[0:64, 1:2]), reads=["col1"], writes=["col2"])
        S.op("dve", lambda E: E.tensor_scalar(CK[2][:, 0:dv], o_ps, COL[0:64, 2:3], None, ALU.mult), reads=[okey, "col2"], writes=["ck2"])
        P4 = PS4b()
        S.op("pe", [lambda E, j=j: E.transpose(P4[:, 512 + j * 64:512 + (j + 1) * 64], CK[2][:, j * 128:(j + 1) * 128], IDB[0:64, 0:64]) for j in range(dv // 128)],
             reads=["ck2", "idb"], writes=[("ps", 4)])
        for j in range(dv // 128):
            S.op("dve", lambda E, j=j: E.scalar_tensor_tensor(OTS[:, j * MB + c * 64:j * MB + (c + 1) * 64], P4[:, 512 + j * 64:512 + (j + 1) * 64],
                                                              ngcols[j], gts[j][:, cs], ALU.mult, ALU.mult),
                 reads=[("ps", 4), "ngt", ("hb", 6 + j)], writes=["ots"])

    def state_update(dS_ps, dkey, dv, decay_col, dkeys, first_chunk):
        if first_chunk:
            S.op("dve", lambda E: E.tensor_copy(SS[:, 0:dv], dS_ps), reads=[dkey], writes=["ss"])
        else:
            S.op("dve", lambda E: E.scalar_tensor_tensor(SS[:, 0:dv], SS[:, 0:dv], decay_col, dS_ps, ALU.mult, ALU.add), reads=[dkey, "ss"] + dkeys, writes=["ss"])
        S.op("act", lambda E: E.activation(SSB[:, 0:dv], SS[:, 0:dv], AF.Copy), reads=["ss"], writes=["ssb"])

    def diag_block(dv, ngcols, first):
        q, k, la, b, d, e1, e2, e3 = (FT[i][:, 0:MB] for i in range(8))
        b3 = b.rearrange("p (c t) -> p c t", t=64)
        d3 = d.rearrange("p (c t) -> p c t", t=64)
        S.op("dve", lambda E: E.tensor_tensor_scan(b, CMASK[:, :], la, 0.0, ALU.mult, ALU.add), reads=["ft2", "cmask"], writes=["ft3"])
        S.op("dve", lambda E: E.tensor_tensor(d3, b3, b3[:, :, 31:32].broadcast_to([128, MB // 64, 64]), ALU.subtract), reads=["ft3"], writes=["ft4"])
        S.op("dve", lambda E: E.tensor_scalar(d, d, 80.0, -80.0, ALU.min, ALU.max), reads=["ft4"], writes=["ft4"])
        S.op("act", lambda E: E.activation(e1, d, AF.Exp), reads=["ft4"], writes=["ft5"])
        S.op("act", lambda E: E.activation(e2, d, AF.Exp, scale=-1.0), reads=["ft4"], writes=["ft6"])
        S.op("act", lambda E: E.activation(e3, b, AF.Exp), reads=["ft3"], writes=["ft7"])
        S.op("pool", lambda E: E.tensor_tensor(HB[0][:, :], q, e1, ALU.mult), reads=["ft0", "ft5"], writes=[("hb", 0)])
        S.op("pool", lambda E: E.tensor_tensor(HB[1][:, :], k, e2, ALU.mult), reads=["ft1", "ft6"], writes=[("hb", 1)])
        S.op("pool", lambda E: E.tensor_tensor(HB[2][:, :], q, e3, ALU.mult), reads=["ft0", "ft7"], writes=[("hb", 2)])
        S.op("dve", lambda E: E.tensor_tensor(d3, b3[:, :, 63:64].broadcast_to([128, MB // 64, 64]), b3, ALU.subtract), reads=["ft3", "ft5", "ft6"], writes=["ft4"])
        S.op("act", lambda E: E.activation(e1, d, AF.Exp), reads=["ft4", ("hb", 0)], writes=["ft5"])
        S.op("pool", lambda E: E.tensor_tensor(HB[3][:, :], k, e1, ALU.mult), reads=["ft1", "ft5"], writes=[("hb", 3)])
        P4 = PS4b()
        nv = dv // 128
        for c in range(MB // 64):
            cs = slice(c * 64, (c + 1) * 64)
            fns = [lambda E, cs=cs: E.transpose(P4[0:64, 0:128], HB[3][:, cs], IDB[:, :])]
            for j in range(nv):
                fns.append(lambda E, cs=cs, j=j: E.transpose(P4[0:64, 128 + j * 128:256 + j * 128], HB[4 + j][:, cs], IDB[:, :]))
            S.op("pe", fns, reads=[("hb", 3), "idb"] + [("hb", 4 + j) for j in range(nv)], writes=[("ps", 4)])
            S.op("act", lambda E: E.activation(CK[0][:, 0:128 + dv], P4[0:64, 0:128 + dv], AF.Copy), reads=[("ps", 4)], writes=["ck0"])
            S.op("pe", lambda E, cs=cs: E.matmul(PS[5][0:64, 0:64], HB[1][:, cs], HB[0][:, cs], start=True, stop=True), reads=[("hb", 0), ("hb", 1)], writes=[("ps", 5)])
            S.op("dve", lambda E: E.tensor_tensor(CK[1][:, 0:64], PS[5][0:64, 0:64], MASKT[:, :], ALU.mult), reads=[("ps", 5), "maskt"], writes=["ck1"])
            fc = first and c == 0
            if fc:
                S.op("pe", lambda E: E.matmul(PS[6][0:64, 0:dv], CK[1][:, 0:64], CK[0][:, 128:128 + dv], start=True, stop=True), reads=["ck1", "ck0"], writes=[("ps", 6)])
            else:
                S.op("pe", [lambda E: E.matmul(PS[6][0:64, 0:dv], CK[1][:, 0:64], CK[0][:, 128:128 + dv], start=True, stop=False),
                            lambda E, cs=cs: E.matmul(PS[6][0:64, 0:dv], HB[2][:, cs], SSB[:, 0:dv], start=False, stop=True)],
                     reads=["ck1", "ck0", ("hb", 2), "ssb"], writes=[("ps", 6)])
            S.op("pe", lambda E: E.matmul(PS[7][:, 0:dv], CK[0][:, 0:128], CK[0][:, 128:128 + dv], start=True, stop=True), reads=["ck0"], writes=[("ps", 7)])
            state_update(PS[7][:, 0:dv], ("ps", 7), dv, FT[7][:, c * 64 + 63:c * 64 + 64], ["ft7"], fc)
            rms_gate_out(PS[6][0:64, 0:dv], ("ps", 6), dv, [HB[6 + j] for j in range(nv)], ngcols, c)

    def gdn_block(first):
        P4 = PS4b()
        G128, E128 = FT[6], FT[7]
        for c in range(MB // 64):
            cs = slice(c * 64, (c + 1) * 64)
            fc = first and c == 0
            S.op("pe", lambda E, cs=cs: E.matmul(PS[5][0:64, 256:258], FT[9][0:64, cs], SEL2[:, :], start=True, stop=True), reads=["ft9", "sel2"], writes=[("ps", 5)])
            S.op("dve", lambda E: E.tensor_copy(COL[0:64, 4:6], PS[5][0:64, 256:258]), reads=[("ps", 5)], writes=["col4"])
            bcol, gcol = COL[0:64, 4:5], COL[0:64, 5:6]
            S.op("act", lambda E: E.activation(COL[0:64, 6:7], gcol, AF.Exp), reads=["col4"], writes=["col6"])
            S.op("act", lambda E, c=c: E.activation(COL[0:64, 7:8], gcol, AF.Exp, scale=-1.0, bias=G128[0:64, c * 64 + 63:c * 64 + 64]), reads=["col4", "ft6"], writes=["col7"])
            S.op("dve", lambda E: E.tensor_tensor(COL[0:64, 8:9], bcol, COL[0:64, 6:7], ALU.mult), reads=["col4", "col6"], writes=["col8"])
            S.op("pe", [lambda E, cs=cs: E.transpose(P4[0:64, 0:128], HB[1][:, cs], IDB[:, :]),
                        lambda E, cs=cs: E.transpose(P4[0:64, 128:256], HB[4][:, cs], IDB[:, :])], reads=[("hb", 1), ("hb", 4), "idb"], writes=[("ps", 4)])
            S.op("act", lambda E: E.activation(CK[0][:, 0:256], P4[0:64, 0:256], AF.Copy), reads=[("ps", 4)], writes=["ck0"])
            S.op("pool", lambda E: E.tensor_scalar(CK[3][:, 0:128], CK[0][:, 128:256], bcol, None, ALU.mult), reads=["ck0", "col4"], writes=["ck3a"])
            S.op("pool", lambda E: E.tensor_scalar(CK[3][:, 128:256], CK[0][:, 0:128], COL[0:64, 8:9], None, ALU.mult), reads=["ck0", "col8"], writes=["ck3b"])
            S.op("pool", lambda E: E.tensor_scalar(CK[4][:, 0:128], CK[0][:, 0:128], COL[0:64, 7:8], None, ALU.mult), reads=["ck0", "col7"], writes=["ck4a"])
            S.op("pe", lambda E, cs=cs: E.matmul(PS[5][0:64, 0:64], HB[1][:, cs], HB[1][:, cs], start=True, stop=True), reads=[("hb", 1)], writes=[("ps", 5)])
            S.op("dve", lambda E, cs=cs: E.tensor_scalar(CF[0][:, :], G128[0:64, cs], gcol, 0.0, ALU.subtract, ALU.max), reads=["ft6", "col4"], writes=["cf0"])
            S.op("act", lambda E: E.activation(CF[0][:, :], CF[0][:, :], AF.Exp, scale=-1.0), reads=["cf0"], writes=["cf0"])
            S.op("dve", lambda E: E.tensor_tensor(CF[1][:, :], PS[5][0:64, 0:64], CF[0][:, :], ALU.mult), reads=[("ps", 5), "cf0"], writes=["cf1"])
            S.op("dve", lambda E: E.tensor_tensor(CF[1][:, :], CF[1][:, :], MASKN[:, :], ALU.mult), reads=["cf1", "maskn"], writes=["cf1"])
            S.op("dve", lambda E: E.tensor_scalar(CF[1][:, :], CF[1][:, :], bcol, None, ALU.mult), reads=["cf1", "col4"], writes=["cf1"])
            S.op("pe", lambda E: E.transpose(PS[7][0:64, 0:64], CF[1][:, :], IDF[0:64, 0:64]), reads=["cf1", "idf"], writes=[("ps", 7)])
            S.op("act", lambda E: E.activation(CF[2][:, :], PS[7][0:64, 0:64], AF.Copy), reads=[("ps", 7)], writes=["cf2"])
            S.op("dve", lambda E: E.tensor_tensor(CF[3][:, :], CF[2][:, :], IDF[0:64, 0:64], ALU.add), reads=["cf2", "idf"], writes=["cf3"])
            Pi, PTi, TTi = 1, 2, 3
            free = [4, 5, 6, 7, 8, 9, 10, 11]
            for step in range(5):
                Pn = free.pop(0)
                S.op("pe", lambda E, PTi=PTi, Pi=Pi: E.matmul(PS[7][0:64, 0:64], CF[PTi][:, :], CF[Pi][:, :], start=True, stop=True), reads=["cf%d" % PTi, "cf%d" % Pi], writes=[("ps", 7)])
                S.op("act", lambda E, Pn=Pn: E.activation(CF[Pn][:, :], PS[7][0:64, 0:64], AF.Copy), reads=[("ps", 7)], writes=["cf%d" % Pn])
                if step < 4:
                    PTn = free.pop(0)
                    S.op("pe", lambda E, PTi=PTi, Pi=Pi: E.matmul(PS[5][0:64, 64:128], CF[Pi][:, :], CF[PTi][:, :], start=True, stop=True), reads=["cf%d" % PTi, "cf%d" % Pi], writes=[("ps", 5)])
                    S.op("dve", lambda E, PTn=PTn: E.tensor_copy(CF[PTn][:, :], PS[5][0:64, 64:128]), reads=[("ps", 5)], writes=["cf%d" % PTn])
                TTn = free.pop(0)
                S.op("pe", lambda E, Pn=Pn, TTi=TTi: E.matmul(PS[7][0:64, 128:192], CF[Pn][:, :], CF[TTi][:, :], start=True, stop=True), reads=["cf%d" % Pn, "cf%d" % TTi], writes=[("ps", 7)])
                S.op("dve", lambda E, TTn=TTn, TTi=TTi: E.tensor_tensor(CF[TTn][:, :], CF[TTi][:, :], PS[7][0:64, 128:192], ALU.add), reads=[("ps", 7), "cf%d" % TTi], writes=["cf%d" % TTn])
                free.extend([Pi, TTi] + ([PTi] if step < 4 else []))
                Pi, TTi = Pn, TTn
                if step < 4:
                    PTi = PTn
            S.op("act", lambda E, TTi=TTi: E.activation(CK[5][:, 0:64], CF[TTi][:, :], AF.Copy), reads=["cf%d" % TTi], writes=["ck5"])
            S.op("pe", lambda E: E.matmul(PS[7][:, 256:320], CK[3][:, 128:256], CK[5][:, 0:64], start=True, stop=True), reads=["ck3b", "ck5"], writes=[("ps", 7)])
            S.op("act", lambda E, cs=cs: E.activation(HB[3][:, cs], PS[7][:, 256:320], AF.Copy, scale=-1.0), reads=[("ps", 7)], writes=[("hb", 3)])
            if fc:
                S.op("pe", lambda E: E.matmul(PS[6][0:64, 0:128], CK[5][:, 0:64], CK[3][:, 0:128], start=True, stop=True), reads=["ck5", "ck3a"], writes=[("ps", 6)])
            else:
                S.op("pe", [lambda E: E.matmul(PS[6][0:64, 0:128], CK[5][:, 0:64], CK[3][:, 0:128], start=True, stop=False),
                            lambda E, cs=cs: E.matmul(PS[6][0:64, 0:128], HB[3][:, cs], SSB[:, 0:128], start=False, stop=True)],
                     reads=["ck5", "ck3a", ("hb", 3), "ssb"], writes=[("ps", 6)])
            S.op("act", lambda E: E.activation(CK[4][:, 128:256], PS[6][0:64, 0:128], AF.Copy), reads=[("ps", 6)], writes=["ck4b"])
            S.op("pe", lambda E, cs=cs: E.matmul(PS[5][0:64, 128:192], HB[1][:, cs], HB[0][:, cs], start=True, stop=True), reads=[("hb", 0), ("hb", 1)], writes=[("ps", 5)])
            S.op("dve", lambda E, cs=cs: E.tensor_scalar(CF[0][:, :], G128[0:64, cs], gcol, 0.0, ALU.subtract, ALU.min), reads=["ft6", "col4", "cf1"], writes=["cf0"])
            S.op("act", lambda E: E.activation(CF[0][:, :], CF[0][:, :], AF.Exp), reads=["cf0"], writes=["cf0"])
            S.op("dve", lambda E: E.tensor_tensor(CF[0][:, :], PS[5][0:64, 128:192], CF[0][:, :], ALU.mult), reads=[("ps", 5), "cf0"], writes=["cf0"])
            S.op("dve", lambda E: E.tensor_tensor(CK[1][:, 0:64], CF[0][:, :], MASKT[:, :], ALU.mult), reads=["cf0", "maskt"], writes=["ck1"])
            if fc:
                S.op("pe", lambda E: E.matmul(PS[6][0:64, 256:384], CK[1][:, 0:64], CK[4][:, 128:256], start=True, stop=True), reads=["ck1", "ck4b"], writes=[("ps", 6)])
            else:
                S.op("pe", [lambda E, cs=cs: E.matmul(PS[6][0:64, 256:384], HB[2][:, cs], SSB[:, 0:128], start=True, stop=False),
                            lambda E: E.matmul(PS[6][0:64, 256:384], CK[1][:, 0:64], CK[4][:, 128:256], start=False, stop=True)],
                     reads=["ck1", "ck4b", ("hb", 2), "ssb"], writes=[("ps", 6)])
            S.op("pe", lambda E: E.matmul(PS[7][:, 320:448], CK[4][:, 0:128], CK[4][:, 128:256], start=True, stop=True), reads=["ck4a", "ck4b"], writes=[("ps", 7)])
            state_update(PS[7][:, 320:448], ("ps", 7), 128, E128[:, c * 64 + 63:c * 64 + 64], ["ft7"], fc)
            rms_gate_out(PS[6][0:64, 256:384], ("ps", 6), 128, [HB[6]], [NGT[:, 1:2]], c)

    pidc = {}

    def pid_off(E):
        if "v" not in pidc:
            pidc["v"] = E.snap(E.partition_id() * TC)
        return pidc["v"]

    def mixer(l):
        S.allgather(xT_loc, xT_all, reads=[("xtl", b) for b in range(NB)], writes=["xtall"])
        nblk = NT // MB
        bpr = TC // MB
        bps = (NT // 2) // MB
        S.dma("sp", NGT[:, 0:1], ngA_in[l], writes=["ngt"])
        S.dma("sp", NGT[:, 1:2], ngB_in[l], writes=["ngt"])
        S.dma("sp", NGT[:, 2:4], ngC_in[l], writes=["ngt"])
        S.dma("sp", GLAW[:, :], glaw_in[l], writes=["glaw"])
        S.op("act", lambda E: E.activation(GLAWB[:, :], GLAW[:, :], AF.Copy), reads=["glaw"], writes=["glawb"])
        S.dma("sp", GLAB[:, 0:1], glab_in[l], writes=["glab"])
        S.op("dve", lambda E: E.tensor_scalar(GLAB[:, 1:2], GLAB[:, 0:1], -1.0, None, ALU.mult), reads=["glab"], writes=["glab"])
        row0 = {0: 0, 1: 128, 2: 256, 3: 384}
        for si, (off, ncols) in enumerate(WMIX_GROUPS):
            stype = "ABBC"[si]
            WS3 = G[:, 0:32 * ncols].rearrange("p (k c) -> p k c", k=32)
            for q4 in range(4):
                S.dma("sp", G[:, q4 * 8 * ncols:(q4 + 1) * 8 * ncols], wmixb[l][:, off + q4 * 8 * ncols: off + (q4 + 1) * 8 * ncols],
                      reads=[("wmixb", l, off)], writes=[kXA, kG1])
            if stype == "B":
                bs = si - 1
                S.dma("sp", CONVW[:, :], convw_in[l, bs], writes=["convw"])
                S.dma("sp", BSC[:, 0:2], bsc_in[l, bs], writes=["bsc"])
                S.op("act", lambda E: E.activation(BSC[32:33, 2:3], BSC[32:33, 0:1], AF.Exp), reads=["bsc"], writes=["bsc"])
                S.op("dve", lambda E: E.tensor_scalar(BSC[32:33, 2:3], BSC[32:33, 2:3], -1.0, None, ALU.mult), reads=["bsc"], writes=["bsc"])
                S.op("pool", lambda E: E.memset(FT[9][:, :], 0.0), reads=["ft9"], writes=["ft9"])
            for blk in range(nblk):
                rank, lb_ = blk // bpr, blk % bpr
                first = (blk % bps == 0)
                xi = blk % 2
                XT3 = RING[:, xi * 2 * SLOT:(xi * 2 + 2) * SLOT].rearrange("p (k t) -> p k t", k=32)
                xk = [("ring", 2 * xi), ("ring", 2 * xi + 1)]
                S.dma("sp", XT3, xT_all[rank * D:(rank + 1) * D, lb_ * MB:(lb_ + 1) * MB].rearrange("(k p) t -> p k t", p=128), reads=["xtall"], writes=xk)

                def proj(c0, n, XT3=XT3, xk=xk, WS3=WS3):
                    ps, pk = psum_main()
                    S.op("pe", [lambda E, ps=ps, k=k: E.matmul(ps[0:n, 0:MB], WS3[:, k, c0:c0 + n], XT3[:, k, :], start=(k == 0), stop=(k == KC - 1)) for k in range(KC)],
                         reads=[kXA, kG1] + xk, writes=[pk])
                    return ps[0:n, 0:MB], pk

                if stype == "A":
                    ps, pk = proj(0, 128)
                    S.op("act", lambda E, ps=ps: E.activation(FT[0][:, 0:MB], ps, AF.Silu), reads=[pk], writes=["ft0"])
                    ps, pk = proj(128, 128)
                    S.op("act", lambda E, ps=ps: E.activation(FT[8][:, 0:MB], ps, AF.Sigmoid), reads=[pk], writes=["ft8"])
                    S.op("dve", lambda E: E.tensor_scalar(FT[8][:, 0:MB], FT[8][:, 0:MB], LBT[:, 3 * NL + l:3 * NL + l + 1], LBT[:, 2 * NL + l:2 * NL + l + 1], ALU.mult, ALU.add),
                         reads=["ft8", "lbt"], writes=["ft8"])
                    S.op("act", lambda E: E.activation(FT[2][:, 0:MB], FT[8][:, 0:MB], AF.Ln), reads=["ft8"], writes=["ft2"])
                    S.op("dve", lambda E: E.tensor_scalar(FT[1][:, 0:MB], FT[8][:, 0:MB], -1.0, 1.0, ALU.mult, ALU.add), reads=["ft8"], writes=["ft1"])
                    ps, pk = proj(256, 128)
                    S.op("act", lambda E, ps=ps: E.activation(HB[4][:, :], ps, AF.Copy), reads=[pk], writes=[("hb", 4)])
                    ps, pk = proj(384, 128)
                    S.op("act", lambda E, ps=ps: E.activation(HB[6][:, :], ps, AF.Silu), reads=[pk], writes=[("hb", 6)])
                    diag_block(128, [NGT[:, 0:1]], first)
                    nrow = 1
                elif stype == "C":
                    ps, pk = proj(0, 128)
                    S.op("act", lambda E, ps=ps: E.activation(FT[0][:, 0:MB], ps, AF.Copy, scale=128.0 ** -0.5), reads=[pk], writes=["ft0"])
                    ps, pk = proj(128, 128)
                    S.op("act", lambda E, ps=ps: E.activation(FT[1][:, 0:MB], ps, AF.Copy), reads=[pk], writes=["ft1"])
                    for j in range(2):
                        ps, pk = proj(256 + j * 128, 128)
                        S.op("act", lambda E, ps=ps, j=j: E.activation(HB[4 + j][:, :], ps, AF.Copy), reads=[pk], writes=[("hb", 4 + j)])
                        ps, pk = proj(512 + j * 128, 128)
                        S.op("act", lambda E, ps=ps, j=j: E.activation(HB[6 + j][:, :], ps, AF.Silu), reads=[pk], writes=[("hb", 6 + j)])
                    ps, pk = proj(768, 16)
                    S.op("act", lambda E, ps=ps: E.activation(HB[8][0:16, :], ps, AF.Copy), reads=[pk], writes=[("hb", 8)])
                    ps2, pk2 = psum_main()
                    S.op("pe", lambda E, ps2=ps2: E.matmul(ps2[:, 0:MB], GLAWB[:, :], HB[8][0:16, :], start=True, stop=True), reads=["glawb", ("hb", 8)], writes=[pk2])
                    S.op("act", lambda E, ps2=ps2: E.activation(FT[8][:, 0:MB], ps2[:, 0:MB], AF.Exp, scale=-1.0, bias=GLAB[:, 1:2]), reads=[pk2, "glab"], writes=["ft8"])
                    S.op("act", lambda E: E.activation(FT[8][:, 0:MB], FT[8][:, 0:MB], AF.Ln, bias=1.0), reads=["ft8"], writes=["ft8"])
                    S.op("dve", lambda E: E.tensor_scalar(FT[2][:, 0:MB], FT[8][:, 0:MB], -1.0 / 16.0, None, ALU.mult), reads=["ft8"], writes=["ft2"])
                    diag_block(256, [NGT[:, 2:3], NGT[:, 3:4]], first)
                    nrow = 2
                else:
                    for j, dst in enumerate((0, 1, 4)):
                        CV = FT[j]
                        ps, pk = proj(j * 128, 128)
                        if first:
                            S.op("pool", lambda E, CV=CV: E.memset(CV[:, 0:3], 0.0), reads=["ft%d" % j], writes=["ft%d" % j])
                        S.op("act", lambda E, ps=ps, CV=CV: E.activation(CV[:, 3:3 + MB], ps, AF.Copy), reads=[pk], writes=["ft%d" % j])
                        acc = FT[3][:, 0:MB]
                        S.op("dve", lambda E, CV=CV, j=j: E.tensor_scalar(acc, CV[:, 0:MB], CONVW[:, j * 4:j * 4 + 1], None, ALU.mult), reads=["ft%d" % j, "convw"], writes=["ft3"])
                        for tp in range(1, 4):
                            S.op("dve", lambda E, CV=CV, j=j, tp=tp: E.scalar_tensor_tensor(acc, CV[:, tp:tp + MB], CONVW[:, j * 4 + tp:j * 4 + tp + 1], acc, ALU.mult, ALU.add),
                                 reads=["ft%d" % j, "convw", "ft3"], writes=["ft3"])
                        S.op("pool", lambda E, CV=CV: E.tensor_copy(FT[8][:, 0:3], CV[:, MB:MB + 3]), reads=["ft%d" % j], writes=["ft8"])
                        S.op("pool", lambda E, CV=CV: E.tensor_copy(CV[:, 0:3], FT[8][:, 0:3]), reads=["ft8", "ft%d" % j], writes=["ft%d" % j])
                        if j == 2:
                            S.op("act", lambda E: E.activation(HB[4][:, :], acc, AF.Silu), reads=["ft3"], writes=[("hb", 4)])
                        else:
                            S.op("act", lambda E: E.activation(FT[4][:, 0:MB], acc, AF.Silu), reads=["ft3"], writes=["ft4"])
                            S.op("act", lambda E: E.activation(FT[5][:, 0:MB], FT[4][:, 0:MB], AF.Square), reads=["ft4"], writes=["ft5"])
                            S.op("pe", lambda E: E.matmul(PS[5][:, 0:MB], ONF[:, :], FT[5][:, 0:MB], start=True, stop=True), reads=["ft5", "onf"], writes=[("ps", 5)])
                            S.op("act", lambda E: E.activation(FT[5][:, 0:MB], PS[5][:, 0:MB], AF.Sqrt, bias=RMS_EPS), reads=[("ps", 5)], writes=["ft5"])
                            S.op("dve", lambda E: E.reciprocal(FT[5][:, 0:MB], FT[5][:, 0:MB]), reads=["ft5"], writes=["ft5"])
                            sc = (128.0 ** -0.5) if j == 0 else 1.0
                            S.op("dve", lambda E, dst=dst, sc=sc: E.scalar_tensor_tensor(HB[dst][:, :], FT[4][:, 0:MB], sc, FT[5][:, 0:MB], ALU.mult, ALU.mult),
                                 reads=["ft4", "ft5"], writes=[("hb", dst)])
                    ps, pk = proj(384, 128)
                    S.op("act", lambda E, ps=ps: E.activation(HB[6][:, :], ps, AF.Silu), reads=[pk], writes=[("hb", 6)])
                    ps, pk = proj(512, 64)
                    S.op("act", lambda E, ps=ps: E.activation(FT[9][0:1, 0:MB], ps[0:1, :], AF.Sigmoid), reads=[pk], writes=["ft9"])
                    S.op("act", lambda E, ps=ps: E.activation(FT[8][32:33, 0:MB], ps[32:33, :], AF.Exp, bias=BSC[32:33, 1:2]), reads=[pk, "bsc"], writes=["ft8"])
                    S.op("act", lambda E: E.activation(FT[8][32:33, 0:MB], FT[8][32:33, 0:MB], AF.Ln, bias=1.0), reads=["ft8"], writes=["ft8"])
                    S.op("dve", lambda E: E.tensor_scalar(FT[8][32:33, 0:MB], FT[8][32:33, 0:MB], BSC[32:33, 2:3], None, ALU.mult), reads=["ft8", "bsc"], writes=["ft8"])
                    S.op("dve", lambda E: E.tensor_tensor_scan(FT[9][32:33, 0:MB], CMASK[32:33, :], FT[8][32:33, 0:MB], 0.0, ALU.mult, ALU.add), reads=["ft8", "cmask"], writes=["ft9"])
                    S.op("pe", lambda E: E.matmul(PS[5][:, 0:MB], ONF[32:33, :], FT[9][32:33, 0:MB], start=True, stop=True), reads=["ft9", "onf"], writes=[("ps", 5)])
                    S.op("dve", lambda E: E.tensor_copy(FT[6][:, 0:MB], PS[5][:, 0:MB]), reads=[("ps", 5)], writes=["ft6"])
                    S.op("act", lambda E: E.activation(FT[7][:, 0:MB], FT[6][:, 0:MB], AF.Exp), reads=["ft6"], writes=["ft7"])
                    S.op("pool", lambda E: E.tensor_tensor(HB[2][:, :], HB[0][:, :], FT[7][:, 0:MB], ALU.mult), reads=[("hb", 0), "ft7"], writes=[("hb", 2)])
                    gdn_block(first)
                    nrow = 1
                for j in range(nrow):
                    S.dma("sp", oT_part[row0[si] + j * 128: row0[si] + (j + 1) * 128, blk * MB:(blk + 1) * MB], OTS[:, j * MB:(j + 1) * MB], reads=["ots"], writes=[("otp", si, blk, j)])
        allk = [("otp", si, blk, j) for si in range(4) for blk in range(nblk) for j in range(2 if si == 3 else 1)]
        S.allgather(oT_part, oT_all, reads=allk, writes=["otall"])
        for r0 in range(0, NCORE * OROWS, 640):
            S.dma_fn("sp", lambda E, r0=r0: E.dma_start(out=oT_mine[r0:r0 + 640, :], in_=oT_all[r0:r0 + 640, bass.ds(pid_off(E), TC)]),
                     reads=["otall"], writes=[("otm", r0)])
        return [("otm", r0) for r0 in range(0, NCORE * OROWS, 640)]

    for l in range(NL):
        S.dma("sp", WR[:, :], wr_in[l], writes=["wr"])
        S.dma("sp", BR[:, :], br_in[l], writes=["br"])
        S.dma("sp", BGt[:, :], bg_in[l], writes=["bg"])
        S.dma("sp", BUt[:, :], bu_in[l], writes=["bu"])
        S.dma("pool", BDb[:, :].rearrange("p (a b) -> p a b", b=2048), bd_in[l].rearrange("p (a b) -> p a b", b=2048), writes=["bdb"])
        S.dma("sp", LNG[:, :].rearrange("p (a k) -> p a k", a=3), lng_in[l].rearrange("a p k -> p a k"), writes=["lng"])
        S.dma("sp", LNB[:, :].rearrange("p (a k) -> p a k", a=3), lnb_in[l].rearrange("a p k -> p a k"), writes=["lnb"])

        if "oT" in dbg:
            rows = NCORE * OROWS
            for r0 in range(0, rows, 512):
                S.dma("pool", oT_mine[r0:r0 + 512, :], oT_dbg[l, r0:r0 + 512, :], writes=[("otm", r0)])
            otm_keys = [("otm", r0) for r0 in range(0, rows, 512)]
        else:
            otm_keys = mixer(l)
            if "dump_oT" in dbg and l == 0:
                for r0 in range(0, NCORE * OROWS, 640):
                    S.dma("sp", oT_out[r0:r0 + 640, :], oT_mine[r0:r0 + 640, :], reads=otm_keys, writes=[("oto", r0)])

        wkv = wfull[("wkv", l)]
        S.dma("sp", XBF[:, :], memn_d[:, :], reads=["memn_d"], writes=["xbf"])
        for h in range(4):
            sl, sk = slot()
            S.dma("sp", sl, wkv[:, h * SLOT:(h + 1) * SLOT], reads=[("wf", "wkv", l)], writes=[sk])
            ps, pk = psum_main()
            S.op("pe", [lambda E, ps=ps, sl=sl, k=k: E.matmul(ps[:, 0:256], sl[:, k * 128:(k + 1) * 128], MEMN3[:, k, :],
                                                            start=(k == 0), stop=(k == KC - 1)) for k in range(KC)],
                 reads=[sk, "xbf"], writes=[pk])
            S.op("act", lambda E, ps=ps, h=h: E.activation(KT[:, h * 256:(h + 1) * 256], ps[:, 0:256], AF.Copy), reads=[pk], writes=["kt"])
        vsl = []
        for i in range(4):
            sl, sk = slot()
            S.dma("sp", sl, wkv[:, 4 * SLOT + i * SLOT: 4 * SLOT + (i + 1) * SLOT], reads=[("wf", "wkv", l)], writes=[sk])
            vsl.append((sl, sk))
        for mb in range(2):
            ps, pk = psum_main()
            S.op("pe", [lambda E, ps=ps, k=k, mb=mb, vsl=vsl: E.matmul(ps[:, :], MEMN3[:, k, mb * 128:(mb + 1) * 128],
                                                            vsl[k // 8][0][:, (k % 8) * 512:(k % 8 + 1) * 512],
                                                            start=(k == 0), stop=(k == KC - 1)) for k in range(KC)],
                 reads=[s[1] for s in vsl] + ["xbf"], writes=[pk])
            S.op("act", lambda E, ps=ps, mb=mb: E.activation(VV[:, mb * 512:(mb + 1) * 512], ps[:, :], AF.Copy), reads=[pk], writes=["vv"])

        for b in range(NB):
            tcols = slice(b * BLK, (b + 1) * BLK)
            OTB3 = G1[:, 0:KC * BLK].rearrange("p (j t) -> p j t", j=KC)
            S.dma("sp", OTB3[:, 0:8, :], oT_mine[:, tcols].rearrange("(r o) t -> o r t", o=OROWS)[0:128, :, :],
                  reads=otm_keys, writes=[kG1])
            for s_ in range(2):
                S.dma("sp", OTB3[:, 8 + s_:20:2, :], oT_mine[:, tcols].rearrange("(r o) t -> o r t", o=OROWS)[128 + s_ * 128:256 + s_ * 128, 0:6, :],
                      reads=otm_keys, writes=[("otb", 1 + s_)])
                S.dma("sp", OTB3[:, 20 + s_:32:2, :], oT_mine[:, tcols].rearrange("(r o) t -> o r t", o=OROWS)[384 + s_ * 128:512 + s_ * 128, 0:6, :],
                      reads=otm_keys, writes=[("otb", 3 + s_)])
            otb_keys = [kG1] + [("otb", i) for i in range(1, 5)]
            S.dma("sp", XA3, xresT[:, tcols].rearrange("(k p) t -> p k t", p=128), reads=[("xres", b)], writes=[kXA])
            wout = wfull[("wout", l)]
            for cc in range(KC):
                sl, sk = slot()
                S.dma("sp", sl, wout[cc * 128:(cc + 1) * 128, :], reads=[("wf", "wout", l)], writes=[sk])
                ps, pk = psum_main()
                S.op("pe", [lambda E, ps=ps, sl=sl, j=j: E.matmul(ps[:, 0:BLK], sl[:, j * 128:(j + 1) * 128], OTB3[:, j, :],
                                                                start=(j == 0), stop=(j == KC - 1)) for j in range(KC)],
                     reads=[sk] + otb_keys, writes=[pk])
                S.op("dve", lambda E, ps=ps, cc=cc: E.scalar_tensor_tensor(XA3[:, cc, :], XA3[:, cc, :], ALPHA, ps[:, 0:BLK], ALU.mult, ALU.add),
                     reads=[pk, kXA], writes=[kXA])
            ln_fm(LNG[:, 0:KC], LNB[:, 0:KC], ["lng", "lnb"], XBF3, "xbf")
            if dbg.get("stop") == 1:
                store_final(b)
                continue

            wq = wfull[("wq", l)]
            for h in range(4):
                sl, sk = slot()
                S.dma("sp", sl, wq[:, h * SLOT:(h + 1) * SLOT], reads=[("wf", "wq", l)], writes=[sk])
                ps, pk = psum_main()
                S.op("pe", [lambda E, ps=ps, sl=sl, k=k: E.matmul(ps[:, 0:BLK], sl[:, k * 128:(k + 1) * 128], XBF3[:, k, :],
                                                                start=(k == 0), stop=(k == KC - 1)) for k in range(KC)],
                     reads=[sk, "xbf"], writes=[pk])
                S.op("act", lambda E, ps=ps, h=h: E.activation(QT[:, h * BLK:(h + 1) * BLK], ps[:, 0:BLK], AF.Copy, scale=128.0 ** -0.5),
                     reads=[pk], writes=[("qt", h)])
            for tt in range(NTT):
                for h in range(4):
                    S.op("pe", lambda E, h=h, tt=tt: E.matmul(PS[6][:, 0:256], QT[:, h * BLK + tt * 128:h * BLK + (tt + 1) * 128], KT[:, h * 256:(h + 1) * 256],
                                                              start=True, stop=True), reads=[("qt", h), "kt"], writes=[("ps", 6)])
                    S.op("dve", lambda E: E.reduce_max(SM[:, 0:1], PS[6][:, 0:256], AX.X), reads=[("ps", 6)], writes=["sm0"])
                    S.op("dve", lambda E: E.tensor_scalar(SM[:, 1:2], SM[:, 0:1], -1.0, None, ALU.mult), reads=["sm0"], writes=["sm1"])
                    S.op("act", lambda E: E.activation(PSB[:, :], PS[6][:, 0:256], AF.Exp, bias=SM[:, 1:2], accum_out=SM[:, 2:3]),
                         reads=[("ps", 6), "sm1"], writes=["psb", "sm2"])
                    S.op("dve", lambda E: E.reciprocal(SM[:, 3:4], SM[:, 2:3]), reads=["sm2"], writes=["sm3"])
                    S.op("dve", lambda E: E.tensor_scalar(PN[:, :], PSB[:, :], SM[:, 3:4], None, ALU.mult), reads=["psb", "sm3"], writes=["pn"])
                    PS7b = PS[7][:, 0:128].bitcast(BF16)
                    S.op("pe", [lambda E, mb=mb: E.transpose(PS7b[:, mb * 128:(mb + 1) * 128], PN[:, mb * 128:(mb + 1) * 128], IDB[:, :])
                                for mb in range(2)], reads=["pn", "idb"], writes=[("ps", 7)])
                    S.op("act", lambda E: E.activation(PT[:, :], PS7b, AF.Copy), reads=[("ps", 7)], writes=["pt"])
                    ps, pk = psum_main()
                    S.op("pe", [lambda E, ps=ps, mb=mb, h=h: E.matmul(ps[:, 0:128], VV[:, mb * 512 + h * 128: mb * 512 + (h + 1) * 128], PT[:, mb * 128:(mb + 1) * 128],
                                                                    start=(mb == 0), stop=(mb == 1)) for mb in range(2)],
                         reads=["vv", "pt"], writes=[pk])
                    S.op("act", lambda E, ps=ps, h=h, tt=tt: E.activation(OX[:, h * BLK + tt * 128:h * BLK + (tt + 1) * 128], ps[:, 0:128], AF.Copy),
                         reads=[pk], writes=[("ox", h)])
            wo = wfull[("wo", l)]
            for c8 in range(4):
                sl, sk = slot()
                S.dma("sp", sl, wo[:, c8 * SLOT:(c8 + 1) * SLOT], reads=[("wf", "wo", l)], writes=[sk])
                for ci in range(8):
                    cc = c8 * 8 + ci
                    ps, pk = psum_main()
                    S.op("pe", [lambda E, ps=ps, sl=sl, ci=ci, h=h: E.matmul(ps[:, 0:BLK], sl[:, (ci * 4 + h) * 128:(ci * 4 + h + 1) * 128], OX[:, h * BLK:(h + 1) * BLK],
                                                                           start=(h == 0), stop=(h == 3)) for h in range(4)],
                         reads=[sk] + [("ox", h) for h in range(4)], writes=[pk])
                    S.op("dve", lambda E, ps=ps, cc=cc: E.scalar_tensor_tensor(XA3[:, cc, :], XA3[:, cc, :], ALPHA, ps[:, 0:BLK], ALU.mult, ALU.add),
                         reads=[pk, kXA], writes=[kXA])
            ln_fm(LNG[:, KC:2 * KC], LNB[:, KC:2 * KC], ["lng", "lnb"], XBF3, "xbf")
            if dbg.get("stop") == 2:
                store_final(b)

            WR3 = WR[:, :].rearrange("p (k e) -> p k e", k=KC)
            S.op("pe", [lambda E, k=k: E.matmul(PS[6][0:32, 0:BLK], WR3[:, k, :], XA3[:, k, :], start=(k == 0), stop=(k == KC - 1))
                        for k in range(KC)], reads=["wr", kXA], writes=[("ps", 6)])
            S.op("dve", lambda E: E.tensor_scalar(LGT[:, :], PS[6][0:32, 0:BLK], BR[:, 0:1], None, ALU.add), reads=[("ps", 6), "br"], writes=["lgt"])
            for tt in range(NTT):
                S.op("pe", lambda E, tt=tt: E.transpose(PS[7][:, 0:32], LGT[:, tt * 128:(tt + 1) * 128], IDF[0:32, 0:32]),
                     reads=["lgt", "idf"], writes=[("ps", 7)])
                lg, ex, msk, cm = (LG[:, i * 32:(i + 1) * 32] for i in range(4))
                S.op("dve", lambda E: E.tensor_copy(lg, PS[7][:, 0:32]), reads=[("ps", 7)], writes=["lg0"])
                S.op("dve", lambda E: E.max(SM[:, 8:16], lg), reads=["lg0"], writes=["sm8"])
                S.op("dve", lambda E: E.tensor_scalar(SM[:, 4:5], SM[:, 8:9], -1.0, None, ALU.mult), reads=["sm8"], writes=["sm4"])
                S.op("act", lambda E: E.activation(ex, lg, AF.Exp, bias=SM[:, 4:5]), reads=["lg0", "sm4"], writes=["lg1"])
                S.op("dve", lambda E: E.tensor_scalar(msk, lg, SM[:, 11:12], None, ALU.is_ge), reads=["lg0", "sm8"], writes=["lg2"])
                S.op("dve", lambda E: E.tensor_tensor(ex, ex, msk, ALU.mult), reads=["lg1", "lg2"], writes=["lg1"])
                S.op("dve", lambda E: E.reduce_sum(SM[:, 5:6], ex, AX.X), reads=["lg1"], writes=["sm5"])
                S.op("dve", lambda E: E.reciprocal(SM[:, 6:7], SM[:, 5:6]), reads=["sm5"], writes=["sm6"])
                S.op("dve", lambda E: E.tensor_scalar(cm, ex, SM[:, 6:7], None, ALU.mult), reads=["lg1", "sm6"], writes=["lg3"])
                S.op("pe", lambda E: E.transpose(PS[7][0:32, 128:256], cm, IDF[:, :]), reads=["lg3", "idf"], writes=[("ps", 7)])
                S.op("dve", lambda E, tt=tt: E.tensor_copy(CT[:, tt * 128:(tt + 1) * 128], PS[7][0:32, 128:256]), reads=[("ps", 7)], writes=["ct"])
            S.op("act", lambda E: E.activation(CTB[:, :], CT[:, :], AF.Copy), reads=["ct"], writes=["ctb"])
            wg, wu, wd = wfull[("wg", l)], wfull[("wu", l)], wfull[("wd", l)]
            for e in range(32):
                for fb in range(2):
                    sl, sk = slot()
                    S.dma("sp", sl, wg[e * 128:(e + 1) * 128, fb * SLOT:(fb + 1) * SLOT], reads=[("wf", "wg", l)], writes=[sk])
                    ps, pk = psum_main()
                    S.op("pe", [lambda E, ps=ps, sl=sl, k=k: E.matmul(ps[:, 0:BLK], sl[:, k * 128:(k + 1) * 128], XBF3[:, k, :],
                                                                    start=(k == 0), stop=(k == KC - 1)) for k in range(KC)],
                         reads=[sk, "xbf"], writes=[pk])
                    col = e * 2 + fb
                    S.op("dve", lambda E, ps=ps, col=col: E.tensor_scalar(T1, ps[:, 0:BLK], BGt[:, col:col + 1], 7.0, ALU.add, ALU.min),
                         reads=[pk, "bg"], writes=["ft0"])
                    S.op("act", lambda E: E.activation(T2, T1, AF.Sigmoid, scale=1.702), reads=["ft0"], writes=["ft1"])
                    S.op("pool", lambda E: E.tensor_tensor(T1, T1, T2, ALU.mult), reads=["ft0", "ft1"], writes=["ft0"])
                    sl, sk = slot()
                    S.dma("sp", sl, wu[e * 128:(e + 1) * 128, fb * SLOT:(fb + 1) * SLOT], reads=[("wf", "wu", l)], writes=[sk])
                    ps, pk = psum_main()
                    S.op("pe", [lambda E, ps=ps, sl=sl, k=k: E.matmul(ps[:, 0:BLK], sl[:, k * 128:(k + 1) * 128], XBF3[:, k, :],
                                                                    start=(k == 0), stop=(k == KC - 1)) for k in range(KC)],
                         reads=[sk, "xbf"], writes=[pk])
                    S.op("dve", lambda E, ps=ps, col=col: E.tensor_scalar(T3, ps[:, 0:BLK], BUt[:, col:col + 1], 7.0, ALU.add, ALU.min),
                         reads=[pk, "bu"], writes=["ft2"])
                    S.op("pool", lambda E: E.tensor_scalar(T3, T3, -7.0, 1.0, ALU.max, ALU.add), reads=["ft2"], writes=["ft2"])
                    S.op("pool", lambda E: E.tensor_tensor(T1, T1, T3, ALU.mult), reads=["ft0", "ft2"], writes=["ft0"])
                    S.op("pe", lambda E, e=e: E.matmul(PS[6][:, 0:BLK], SELB[:, e * 128:(e + 1) * 128], CTB[:, :], start=True, stop=True),
                         reads=["selb", "ctb"], writes=[("ps", 6)])
                    S.op("dve", lambda E, col=col: E.tensor_tensor(HT3[:, col, :], T1, PS[6][:, 0:BLK], ALU.mult),
                         reads=["ft0", ("ps", 6)], writes=[kG1])
            for dc in range(KC):
                sls = []
                for hf in range(2):
                    sl, sk = slot()
                    S.dma("sp", sl, wd[dc * 128:(dc + 1) * 128, hf * SLOT:(hf + 1) * SLOT], reads=[("wf", "wd", l)], writes=[sk])
                    sls.append((sl, sk))
                ps, pk = psum_main()
                for hf in range(2):
                    fns = [lambda E, ps=ps, i=i, sl=sls[hf][0], hf=hf: E.matmul(ps[:, 0:BLK], sl[:, i * 128:(i + 1) * 128], HT3[:, hf * 32 + i, :],
                                                                         start=(hf == 0 and i == 0), stop=False) for i in range(32)]
                    if hf == 1:
                        fns.append(lambda E, ps=ps, dc=dc: E.matmul(ps[:, 0:BLK], BDb[:, dc * 128:(dc + 1) * 128], CTB[:, :], start=False, stop=True))
                    S.op("pe", fns, reads=[sls[hf][1], kG1, "bdb", "ctb"], writes=[pk])
                S.op("dve", lambda E, ps=ps, dc=dc: E.scalar_tensor_tensor(XA3[:, dc, :], XA3[:, dc, :], ALPHA, ps[:, 0:BLK], ALU.mult, ALU.add),
                     reads=[pk, kXA], writes=[kXA])
            ln_fm(LNG[:, 2 * KC:3 * KC], LNB[:, 2 * KC:3 * KC], ["lng", "lnb"], XBF3, "xbf")

            if dbg.get("stop") == 2:
                continue
            if l < NL - 1:
                S.dma("sp", xresT[:, tcols].rearrange("(k p) t -> p k t", p=128), XA3, reads=[kXA], writes=[("xres", b)])
                S.dma("sp", xT_loc[:, tcols].rearrange("(k p) t -> p k t", p=128), XBF3, reads=["xbf"], writes=[("xtl", b)])
            else:
                store_final(b)
    S.finish([("out", b, tt) for b in range(NB) for tt in range(NTT)])
    es.close()
    return nc, S


def _prep(inp, TC, NL):
    f = np.float32
    x = np.asarray(inp["x"], f)
    B, SEQ, _ = x.shape
    assert B * SEQ == NCORE * TC
    maps = [dict() for _ in range(NCORE)]
    xf = x.reshape(NCORE, TC, D)
    mem = np.asarray(inp["mem"], f)

    def shard(full):
        r = full.shape[1] // NCORE
        return [np.ascontiguousarray(full[:, c * r:(c + 1) * r, :]) for c in range(NCORE)]

    w_out = np.asarray(inp["w_out"], f)[:NL]
    wout_r = w_out.reshape(NL, KC, 128, KC, 128).transpose(0, 3, 2, 1, 4).reshape(NL, 4096, 4096)
    wq = np.asarray(inp["xattn_wq"], f)[:NL]
    wq_r = wq.reshape(NL, KC, 128, 4, 128).transpose(0, 2, 3, 1, 4).reshape(NL, 128, 16384)
    wkv = np.asarray(inp["xattn_wkv"], f)[:NL]
    wk_r = wkv[:, :, 0:512].reshape(NL, KC, 128, 4, 128).transpose(0, 2, 3, 1, 4).reshape(NL, 128, 16384)
    wv_r = wkv[:, :, 512:1024].reshape(NL, KC, 128, 512).transpose(0, 2, 1, 3).reshape(NL, 128, 16384)
    wkv_r = np.concatenate([wk_r, wv_r], axis=2)
    wo = np.asarray(inp["xattn_wo"], f)[:NL]
    wo_r = wo.reshape(NL, 4, 128, KC, 128).transpose(0, 2, 3, 1, 4).reshape(NL, 128, 16384)

    def gu(w):
        return w.reshape(NL, 32, KC, 128, 2, 128).transpose(0, 1, 3, 4, 2, 5).reshape(NL, 4096, 8192)

    wg_r = gu(np.asarray(inp["w_gate"], f)[:NL])
    wu_r = gu(np.asarray(inp["w_up"], f)[:NL])
    wdn = np.asarray(inp["w_down"], f)[:NL]
    wd_r = wdn.reshape(NL, 32, 2, 128, KC, 128).transpose(0, 4, 3, 1, 2, 5).reshape(NL, 4096, 8192)
    shards = {"wout": shard(wout_r), "wq": shard(wq_r), "wkv": shard(wkv_r), "wo": shard(wo_r),
              "wg": shard(wg_r), "wu": shard(wu_r), "wd": shard(wd_r)}

    wr = np.asarray(inp["w_router"], f)[:NL]
    wr_r = np.ascontiguousarray(wr.reshape(NL, KC, 128, 32).transpose(0, 2, 1, 3).reshape(NL, 128, KC * 32))
    br = np.ascontiguousarray(np.asarray(inp["b_router"], f)[:NL].reshape(NL, 32, 1))

    def bcol(bv):
        return np.ascontiguousarray(bv.reshape(NL, 32, 2, 128).transpose(0, 3, 1, 2).reshape(NL, 128, 64))

    bg = bcol(np.asarray(inp["b_gate"], f)[:NL])
    bu = bcol(np.asarray(inp["b_up"], f)[:NL])
    bd = np.ascontiguousarray(np.asarray(inp["b_down"], f)[:NL])

    def lcol(v):
        return v.reshape(NL, KC, 128).transpose(0, 2, 1)

    lng = np.ascontiguousarray(np.stack([lcol(np.asarray(inp[k], f)[:NL]) for k in ("ln_mix_g", "ln_xattn_g", "ln_ffn_g")], axis=1))
    lnb = np.ascontiguousarray(np.stack([lcol(np.asarray(inp[k], f)[:NL]) for k in ("ln_mix_b", "ln_xattn_b", "ln_ffn_b")], axis=1))
    mlg = np.ascontiguousarray(np.asarray(inp["mem_ln_g"], f).reshape(KC, 128).T)
    mlb = np.ascontiguousarray(np.asarray(inp["mem_ln_b"], f).reshape(KC, 128).T)
    ident = np.eye(128, dtype=f)
    sel = np.zeros((32, 32, 128), f)
    for e in range(32):
        sel[e, e, :] = 1.0
    sel = sel.reshape(32, 32 * 128)
    for c in range(NCORE):
        m = maps[c]
        m["x"] = np.ascontiguousarray(xf[c])
        m["mem"] = np.ascontiguousarray(mem[c // 4])
        for n in shards:
            m["sh_" + n] = shards[n][c]
        m.update(wr=wr_r, br=br, bg=bg, bu=bu, bd=bd, lng=lng, lnb=lnb, mlg=mlg, mlb=mlb, ident=ident, sel=sel)

    w_in = np.asarray(inp["w_in"], f)[:NL]
    o_bqkv, o_bz, o_bb, o_ba, o_cq, o_ck, o_cv, o_cr, o_clr = 4096, 8704, 10240, 10252, 10264, 11032, 11800, 13336, 14872
    conv_w = np.asarray(inp["gdn_conv_w"], f)[:NL]
    a_log = np.asarray(inp["gdn_a_log"], f)[:NL]
    dt_b = np.asarray(inp["gdn_dt_bias"], f)[:NL]
    lb_raw = np.asarray(inp["hgrn_lb_raw"], f)[:NL]
    gla_w = np.asarray(inp["gla_w_up"], f)[:NL]
    gla_b = np.asarray(inp["gla_b_up"], f)[:NL]
    cmask = np.ones((128, MB), f)
    cmask[:, ::64] = 0.0
    ii = np.arange(64)
    maskT = (ii[:, None] <= ii[None, :]).astype(f)
    maskN = -(ii[None, :] < ii[:, None]).astype(f)
    sel2 = np.zeros((64, 2), f)
    sel2[0, 0] = 1.0
    sel2[32, 1] = 1.0
    for c in range(NCORE):
        Wc = np.zeros((NL, D, 2448), f)
        Wc[:, :, 0:128] = w_in[:, :, c * 128:(c + 1) * 128]
        Wc[:, :, 128:256] = w_in[:, :, 1024 + c * 128:1024 + (c + 1) * 128]
        Wc[:, :, 256:384] = w_in[:, :, 2048 + c * 128:2048 + (c + 1) * 128]
        Wc[:, :, 384:512] = w_in[:, :, 3072 + c * 128:3072 + (c + 1) * 128]
        convw = np.zeros((NL, 2, 128, 12), f)
        bsc = np.zeros((NL, 2, 64, 2), f)
        glaw = np.zeros((NL, 16, 128), f)
        glab = np.zeros((NL, 128, 1), f)
        if c < 6:
            for s_ in range(2):
                h = 2 * c + s_
                b0 = 512 + s_ * 576
                for j in range(3):
                    Wc[:, :, b0 + j * 128:b0 + (j + 1) * 128] = w_in[:, :, o_bqkv + j * 1536 + h * 128:o_bqkv + j * 1536 + (h + 1) * 128]
                    convw[:, s_, :, j * 4:(j + 1) * 4] = conv_w[:, :, j * 1536 + h * 128:j * 1536 + (h + 1) * 128].transpose(0, 2, 1)
                Wc[:, :, b0 + 384:b0 + 512] = w_in[:, :, o_bz + h * 128:o_bz + (h + 1) * 128]
                Wc[:, :, b0 + 512] = w_in[:, :, o_bb + h]
                Wc[:, :, b0 + 512 + 32] = w_in[:, :, o_ba + h]
                bsc[:, s_, 32, 0] = a_log[:, h]
                bsc[:, s_, 32, 1] = dt_b[:, h]
            h = c
            c0 = 512 + 2 * 576
            Wc[:, :, c0:c0 + 128] = w_in[:, :, o_cq + h * 128:o_cq + (h + 1) * 128]
            Wc[:, :, c0 + 128:c0 + 256] = w_in[:, :, o_ck + h * 128:o_ck + (h + 1) * 128]
            Wc[:, :, c0 + 256:c0 + 512] = w_in[:, :, o_cv + h * 256:o_cv + (h + 1) * 256]
            Wc[:, :, c0 + 512:c0 + 768] = w_in[:, :, o_cr + h * 256:o_cr + (h + 1) * 256]
            Wc[:, :, c0 + 768:c0 + 784] = w_in[:, :, o_clr:o_clr + 16]
            glaw[:] = gla_w[:, :, h * 128:(h + 1) * 128]
            glab[:, :, 0] = gla_b[:, h * 128:(h + 1) * 128]
        parts = []
        for (c0_, nco) in ((0, 512), (512, 576), (1088, 576), (1664, 784)):
            parts.append(Wc[:, :, c0_:c0_ + nco].reshape(NL, KC, 128, nco).transpose(0, 2, 1, 3).reshape(NL, 128, KC * nco))
        m = maps[c]
        m["wmix"] = np.ascontiguousarray(np.concatenate(parts, axis=2))
        m["convw"] = convw
        m["bsc"] = bsc
        m["lbT"] = np.ascontiguousarray(lb_raw[:, c * 128:(c + 1) * 128].T)
        m["ngA"] = np.ascontiguousarray(np.asarray(inp["hgrn_norm_g"], f)[:NL].reshape(NL, 128, 1))
        m["ngB"] = np.ascontiguousarray(np.asarray(inp["gdn_norm_g"], f)[:NL].reshape(NL, 128, 1))
        m["ngC"] = np.ascontiguousarray(np.asarray(inp["gla_norm_g"], f)[:NL].reshape(NL, 2, 128).transpose(0, 2, 1))
        m["glaw"] = glaw
        m["glab"] = glab
        m.update(cmask=cmask, maskT=maskT, maskN=maskN, sel2=sel2)
    return maps


def kernel(**inputs):
    TC, NL = 2048, 4
    nc, _ = build(TC, NL)
    maps = _prep(inputs, TC, NL)
    res = run_bass_kernel_spmd(nc, maps, core_ids=list(range(NCORE)))
    out = np.stack([np.asarray(r["out"], np.float32) for r in res.results], axis=0)
    return out.reshape(2, 4 * TC, D)
```
